# Optimizing a Trainium2 kernel written in Bass

```python
import math
import jax, jax.numpy as jnp
from jax import lax
import numpy as np

D_MODEL = 1024
BATCH = 16
SEQ = 2048
DEPTH = 2

HEAD_DIM = 64
N_HEADS = D_MODEL // HEAD_DIM
A_HEADS = N_HEADS // 2
A_BRANCHES = ((128, 1), (512, 4), (2048, 16))
B_HEADS = N_HEADS - A_HEADS
B_Q_RANK = D_MODEL // 4
IDX_HEADS = 8
IDX_DIM = 32
TOPK_MAX = 256
C_HEADS = N_HEADS
C_KV_HEADS = C_HEADS // 8
C_WINDOW = 128
D_FF = 2816
NUM_BUCKETS = 32
MAX_DISTANCE = 2048
BLOCK = 128
EPS = 1e-6
N_EVEN = (DEPTH + 1) // 2
N_ODD = DEPTH // 2
A_W = A_HEADS * HEAD_DIM
B_W = B_HEADS * HEAD_DIM
EVEN_SIZES = (A_W, A_W, A_W, B_Q_RANK, B_W, B_W, IDX_DIM, IDX_HEADS)
EVEN_IN = sum(EVEN_SIZES)
ODD_SIZES = (C_HEADS * HEAD_DIM, C_KV_HEADS * HEAD_DIM, C_KV_HEADS * HEAD_DIM)
ODD_IN = sum(ODD_SIZES)

kernel_name = "hybrid_dilated_dsa_swa_macaron"

F32 = jnp.float32


def _offsets(sizes):
    out, acc = [], 0
    for s in sizes[:-1]:
        acc += s
        out.append(acc)
    return out


def rmsnorm(x, g):
    xf = x.astype(F32)
    y = xf * lax.rsqrt(jnp.mean(xf * xf, axis=-1, keepdims=True) + EPS)
    return (y * g.astype(F32)).astype(x.dtype)


def swiglu(x, w1, w3, w2):
    return (jax.nn.silu(x @ w1) * (x @ w3)) @ w2


def t5_bucket(dist):
    dist = jnp.maximum(dist, 0)
    max_exact = NUM_BUCKETS // 2
    d_f = jnp.maximum(dist, 1).astype(F32)
    large = max_exact + (jnp.log(d_f / max_exact) / math.log(MAX_DISTANCE / max_exact)
                         * (NUM_BUCKETS - max_exact)).astype(jnp.int32)
    large = jnp.minimum(large, NUM_BUCKETS - 1)
    return jnp.where(dist < max_exact, dist, large)


def rel_bias(table, dist):
    return table.astype(F32)[t5_bucket(dist)]


def local_dist():
    return jnp.arange(BLOCK)[:, None] + BLOCK - jnp.arange(2 * BLOCK)[None, :]


def banded_attention(q, k, v, bias, max_dist):
    n, L, H, dh = q.shape
    hk = k.shape[2]
    g = H // hk
    nb = -(-L // BLOCK)
    lp = nb * BLOCK
    pad = lp - L
    qb = jnp.pad(q, ((0, 0), (0, pad), (0, 0), (0, 0))).reshape(n, nb, BLOCK, hk, g, dh)

    def windows(t):
        tp = jnp.pad(t, ((0, 0), (BLOCK, pad), (0, 0), (0, 0))).reshape(n, nb + 1, BLOCK, hk, dh)
        return jnp.concatenate([tp[:, :-1], tp[:, 1:]], axis=2)

    kw, vw = windows(k), windows(v)
    s = jnp.einsum('nbqhgd,nbshd->nbhgqs', qb, kw, preferred_element_type=F32) * (dh ** -0.5)
    s = s + bias.reshape(hk, g, BLOCK, 2 * BLOCK)[None, None]
    loc = local_dist()
    key_abs = jnp.arange(nb)[:, None, None] * BLOCK + jnp.arange(2 * BLOCK)[None, None, :] - BLOCK
    mask = (loc >= 0)[None] & (loc <= max_dist)[None] & (key_abs >= 0)
    s = jnp.where(mask[None, :, None, None], s, -jnp.inf)
    m = jnp.max(s, axis=-1, keepdims=True)
    p = jnp.exp(s - m)
    den = jnp.sum(p, axis=-1)
    o = jnp.einsum('nbhgqs,nbshd->nbqhgd', p, vw.astype(F32))
    o = o / jnp.transpose(den, (0, 1, 4, 2, 3))[..., None]
    lse = jnp.transpose(m[..., 0] + jnp.log(den), (0, 1, 4, 2, 3))
    o = o.reshape(n, lp, H, dh)[:, :L]
    lse = lse.reshape(n, lp, H)[:, :L]
    return o, lse


def dilated_mixture(q, k, v, table):
    bn, S, H, dh = q.shape
    outs, lses = [], []
    for window, dil in A_BRANCHES:
        ls = S // dil

        def fold(t):
            return t.reshape(bn, ls, dil, H, dh).transpose(0, 2, 1, 3, 4).reshape(bn * dil, ls, H, dh)

        bias = jnp.moveaxis(rel_bias(table, dil * local_dist()), -1, 0)
        o, lse = banded_attention(fold(q), fold(k), fold(v), bias, window // dil)
        outs.append(o.reshape(bn, dil, ls, H, dh).transpose(0, 2, 1, 3, 4).reshape(bn, S, H, dh))
        lses.append(lse.reshape(bn, dil, ls, H).transpose(0, 2, 1, 3).reshape(bn, S, H))
    wts = jax.nn.softmax(jnp.stack(lses), axis=0)
    return jnp.einsum('rbsh,rbshd->bshd', wts, jnp.stack(outs))


def dsa_attention(q, k, v, q_idx, k_idx, w_idx, table, topk):
    bn, S, H, dh = q.shape
    nb = S // BLOCK
    idx_scale = (IDX_DIM ** -0.5) * (IDX_HEADS ** -0.5)
    spos = jnp.arange(S)

    def to_blocks(t):
        return jnp.swapaxes(t.reshape((bn, nb, BLOCK) + t.shape[2:]), 0, 1)

    def one_block(args):
        qb, qib, wb, t0 = args
        tpos = t0 + jnp.arange(BLOCK)
        sc = jax.nn.relu(jnp.einsum('bqhd,bsd->bqhs', qib, k_idx, preferred_element_type=F32))
        score = jnp.einsum('bqhs,bqh->bqs', sc, wb.astype(F32)) * idx_scale
        score = jnp.where((spos[None, :] <= tpos[:, None])[None], score, -jnp.inf)
        _, idx = lax.top_k(score, topk)
        ks = jax.vmap(lambda kk, ii: kk[ii])(k, idx)
        vs = jax.vmap(lambda vv, ii: vv[ii])(v, idx)
        dist = tpos[None, :, None] - idx
        logits = jnp.einsum('bqhd,bqkhd->bqhk', qb, ks, preferred_element_type=F32) * (dh ** -0.5)
        logits = logits + jnp.moveaxis(rel_bias(table, dist), -1, 2)
        logits = jnp.where((dist >= 0)[:, :, None, :], logits, -jnp.inf)
        p = jax.nn.softmax(logits, axis=-1)
        return jnp.einsum('bqhk,bqkhd->bqhd', p, vs.astype(F32))

    t0s = jnp.arange(nb, dtype=jnp.int32) * BLOCK
    out = lax.map(one_block, (to_blocks(q), to_blocks(q_idx), to_blocks(w_idx), t0s))
    return jnp.swapaxes(out, 0, 1).reshape(bn, S, H, dh)


def hybrid_ab_mixer(u, table, w_in, cq_g, wq_b, wq_idx, w_out):
    bn, S, _ = u.shape
    proj = u @ w_in
    qa, ka, va, cq, kb, vb, k_idx, w_idx = jnp.split(proj, _offsets(EVEN_SIZES), axis=-1)
    hs = (bn, S, A_HEADS, HEAD_DIM)
    o_a = dilated_mixture(qa.reshape(hs), ka.reshape(hs), va.reshape(hs), table[:, :A_HEADS])
    cq = rmsnorm(cq, cq_g)
    hb = (bn, S, B_HEADS, HEAD_DIM)
    qb = (cq @ wq_b).reshape(hb)
    q_idx = (cq @ wq_idx).reshape(bn, S, IDX_HEADS, IDX_DIM)
    topk = min(TOPK_MAX, S // 4)
    o_b = dsa_attention(qb, kb.reshape(hb), vb.reshape(hb), q_idx, k_idx, w_idx,
                        table[:, A_HEADS:], topk)
    o = jnp.concatenate([o_a.reshape(bn, S, A_W), o_b.reshape(bn, S, B_W)], axis=-1)
    return o.astype(u.dtype) @ w_out


def swa_sink_mixer(u, table, w_in, sinks, w_out):
    bn, S, _ = u.shape
    q, k, v = jnp.split(u @ w_in, _offsets(ODD_SIZES), axis=-1)
    q = q.reshape(bn, S, C_HEADS, HEAD_DIM)
    k = k.reshape(bn, S, C_KV_HEADS, HEAD_DIM)
    v = v.reshape(bn, S, C_KV_HEADS, HEAD_DIM)
    bias = jnp.moveaxis(rel_bias(table, local_dist()), -1, 0)
    o, lse = banded_attention(q, k, v, bias, C_WINDOW - 1)
    o = o * jax.nn.sigmoid(lse - sinks.astype(F32))[..., None]
    return o.reshape(bn, S, C_HEADS * HEAD_DIM).astype(u.dtype) @ w_out


def setup_inputs(seed: int = 0) -> dict:
    key = jax.random.key(seed)
    ks = jax.random.split(key, 20)
    nrm = lambda k, shape, scale: jax.random.normal(k, shape, F32) * scale
    return {
        "x": nrm(ks[0], (BATCH, SEQ, D_MODEL), 1.0),
        "norm_g": 1.0 + nrm(ks[1], (DEPTH, 3, D_MODEL), 0.02),
        "final_g": 1.0 + nrm(ks[2], (D_MODEL,), 0.02),
        "rel_bias_table": nrm(ks[3], (NUM_BUCKETS, N_HEADS), 0.2),
        "ffn_w1": nrm(ks[4], (DEPTH, 2, D_MODEL, D_FF), D_MODEL ** -0.5),
        "ffn_w3": nrm(ks[5], (DEPTH, 2, D_MODEL, D_FF), D_MODEL ** -0.5),
        "ffn_w2": nrm(ks[6], (DEPTH, 2, D_FF, D_MODEL), D_FF ** -0.5),
        "hyb_w_in": nrm(ks[7], (N_EVEN, D_MODEL, EVEN_IN), D_MODEL ** -0.5),
        "hyb_cq_g": 1.0 + nrm(ks[8], (N_EVEN, B_Q_RANK), 0.02),
        "hyb_wq_b": nrm(ks[9], (N_EVEN, B_Q_RANK, B_W), B_Q_RANK ** -0.5),
        "hyb_wq_idx": nrm(ks[10], (N_EVEN, B_Q_RANK, IDX_HEADS * IDX_DIM), B_Q_RANK ** -0.5),
        "hyb_w_out": nrm(ks[11], (N_EVEN, A_W + B_W, D_MODEL), (A_W + B_W) ** -0.5),
        "swa_w_in": nrm(ks[12], (N_ODD, D_MODEL, ODD_IN), D_MODEL ** -0.5),
        "swa_sinks": nrm(ks[13], (N_ODD, C_HEADS), 1.0),
        "swa_w_out": nrm(ks[14], (N_ODD, C_HEADS * HEAD_DIM, D_MODEL), (C_HEADS * HEAD_DIM) ** -0.5),
    }


def reference(x, norm_g, final_g, rel_bias_table, ffn_w1, ffn_w3, ffn_w2, hyb_w_in, hyb_cq_g,
              hyb_wq_b, hyb_wq_idx, hyb_w_out, swa_w_in, swa_sinks, swa_w_out):
    h = x
    for layer in range(DEPTH):
        g = norm_g[layer]
        h = h + 0.5 * swiglu(rmsnorm(h, g[0]), ffn_w1[layer, 0], ffn_w3[layer, 0], ffn_w2[layer, 0])
        u = rmsnorm(h, g[1])
        i = layer // 2
        if layer % 2 == 0:
            mix = hybrid_ab_mixer(u, rel_bias_table, hyb_w_in[i], hyb_cq_g[i], hyb_wq_b[i],
                                  hyb_wq_idx[i], hyb_w_out[i])
        else:
            mix = swa_sink_mixer(u, rel_bias_table, swa_w_in[i], swa_sinks[i], swa_w_out[i])
        h = h + mix
        h = h + 0.5 * swiglu(rmsnorm(h, g[2]), ffn_w1[layer, 1], ffn_w3[layer, 1], ffn_w2[layer, 1])
    return rmsnorm(h, final_g)
```

```python
import bisect
import math
import numpy as np
import concourse.bass as bass
import concourse.mybir as mybir
from concourse.bass_utils import run_bass_kernel_spmd

F32 = mybir.dt.float32
BF16 = mybir.dt.bfloat16
AF = mybir.ActivationFunctionType
ALU = mybir.AluOpType
AX = mybir.AxisListType

D = 1024
SEQ = 2048
DFF = 2816
NCH = 8
NFC = 22
EPS = 1e-6
NCORES = 8
SEQ_PER_CORE = 2

SAME_ENGINE_SYNC = True


def _esize(dt):
    return 4 if dt in (F32, mybir.dt.int32, mybir.dt.uint32) else (2 if dt in (BF16, mybir.dt.float16) else 1)


def _range(ap):
    es = _esize(ap.dtype)
    steps = ap.ap
    name = ap.tensor.name
    sp = str(ap.space)
    off = ap.offset
    if 'DRAM' in sp.upper() or 'HBM' in sp.upper():
        dims = steps
        base = off
    else:
        pstep = steps[0][0]
        dims = steps[1:]
        base = off % pstep if pstep > 0 else off
    lo = hi = base
    for s, c in dims:
        if s >= 0:
            hi += s * (c - 1)
        else:
            lo += s * (c - 1)
    if 'PSUM' in sp.upper():
        return (name, 0, 2048)
    return (name, lo * es, (hi + 1) * es)


class _Op:
    __slots__ = ('eng', 'fn', 'deps', 'signal', 'sigval', 'semkey', 'idx')

    def __init__(self, eng, fn):
        self.eng = eng
        self.fn = fn
        self.deps = []
        self.signal = False
        self.sigval = None
        self.semkey = None


class Prog:
    ENGS = ('pe', 'dve', 'act', 'pool', 'sp')

    def __init__(self, nc, sems):
        self.nc = nc
        self.free_sems = list(sems)
        self.sem = {}
        self.count = {}
        for e in self.ENGS:
            self.sem[e] = self.free_sems.pop()
            self.count[e] = 0
        self.ops = []
        self.segs = {}
        self.known = {e: {} for e in self.ENGS}
        self.batch = {}
        self.prefetch_keys = set()
        self.nflush = 0

    def _segments(self, name, lo, hi):
        lst = self.segs.setdefault(name, [])
        out = []
        i = 0
        starts = [s[0] for s in lst]
        i = bisect.bisect_right(starts, lo) - 1
        if i < 0:
            i = 0
        cur = lo
        while cur < hi:
            if i < len(lst) and lst[i][1] <= cur:
                i += 1
                continue
            if i >= len(lst) or lst[i][0] >= hi:
                seg = [cur, hi, None, {}]
                lst.insert(i, seg)
                out.append(seg)
                cur = hi
                break
            s = lst[i]
            if s[0] > cur:
                seg = [cur, s[0], None, {}]
                lst.insert(i, seg)
                out.append(seg)
                cur = s[0]
                i += 1
                continue
            if s[0] < cur:
                left = [s[0], cur, s[2], dict(s[3])]
                s[0] = cur
                lst.insert(i, left)
                i += 1
            if s[1] > hi:
                right = [hi, s[1], s[2], dict(s[3])]
                s[1] = hi
                lst.insert(i + 1, right)
            out.append(s)
            cur = s[1]
            i += 1
        return out

    def _add(self, eng, fn, reads, writes, semkey=None):
        op = _Op(eng, fn)
        op.semkey = semkey
        deps = {}
        rk = semkey if semkey is not None else eng
        rr = [_range(ap) for ap in reads]
        wr = [_range(ap) for ap in writes]
        for name, lo, hi in rr:
            for s in self._segments(name, lo, hi):
                if s[2] is not None:
                    deps[id(s[2])] = s[2]
        for name, lo, hi in wr:
            for s in self._segments(name, lo, hi):
                if s[2] is not None:
                    deps[id(s[2])] = s[2]
                for r in s[3].values():
                    deps[id(r)] = r
        for name, lo, hi in rr:
            for s in self._segments(name, lo, hi):
                s[3][rk] = op
        for name, lo, hi in wr:
            for s in self._segments(name, lo, hi):
                s[2] = op
                s[3] = {}
        for d in deps.values():
            if d.fn is None:
                continue
            if semkey is not None and d.semkey == semkey and d.sigval is None:
                continue
            if d.semkey is None:
                if d.eng == eng and (eng == 'pe' or not SAME_ENGINE_SYNC):
                    continue
                d.signal = True
            op.deps.append(d)
        self.ops.append(op)
        return op

    def mm(self, out, lhsT, rhs, start=True, stop=True, **kw):
        return self._add('pe', lambda e: e.matmul(out, lhsT, rhs, start=start, stop=stop, **kw),
                         [lhsT, rhs], [out])

    def tr(self, out, in_, ident):
        return self._add('pe', lambda e: e.transpose(out, in_, ident), [in_, ident], [out])

    def act(self, out, in_, func, scale=1.0, bias=0.0, accum_out=None):
        reads = [in_]
        writes = [out]
        if not isinstance(scale, (int, float)):
            reads.append(scale)
        if not isinstance(bias, (int, float)):
            reads.append(bias)
        kw = {}
        if accum_out is not None:
            writes.append(accum_out)
            kw['accum_out'] = accum_out
        return self._add('act', lambda e: e.activation(out, in_, func, bias=bias, scale=scale, **kw),
                         reads, writes)

    def tt(self, eng, out, in0, in1, op):
        return self._add(eng, lambda e: e.tensor_tensor(out, in0, in1, op), [in0, in1], [out])

    def ts(self, eng, out, in0, s1, op0, s2=None, op1=ALU.bypass, accum_out=None):
        reads = [in0]
        writes = [out]
        if not isinstance(s1, (int, float)):
            reads.append(s1)
        if s2 is not None and not isinstance(s2, (int, float)):
            reads.append(s2)
        kw = {}
        if accum_out is not None:
            writes.append(accum_out)
            kw['accum_out'] = accum_out
        return self._add(eng, lambda e: e.tensor_scalar(out, in0, s1, s2, op0, op1, **kw), reads, writes)

    def stt(self, out, in0, scalar, in1, op0, op1):
        reads = [in0, in1]
        if not isinstance(scalar, (int, float)):
            reads.append(scalar)
        return self._add('dve', lambda e: e.scalar_tensor_tensor(out, in0, scalar, in1, op0, op1),
                         reads, [out])

    def copy(self, eng, out, in_):
        if eng == 'act':
            return self._add('act', lambda e: e.activation(out, in_, AF.Copy), [in_], [out])
        return self._add(eng, lambda e: e.tensor_copy(out, in_), [in_], [out])

    def memset(self, eng, ap, val):
        return self._add(eng, lambda e: e.memset(ap, val), [], [ap])

    def recip(self, out, in_):
        return self._add('dve', lambda e: e.reciprocal(out, in_), [in_], [out])

    def reduce(self, out, in_, op, absval=False):
        return self._add('dve', lambda e: e.tensor_reduce(out, in_, AX.X, op,
                                                          apply_absolute_value=absval),
                         [in_], [out])

    def dma(self, eng, out, in_, key, last=True, prefetch=False, **kw):
        op = self._add(eng, lambda e: e.dma_start(out=out, in_=in_, **kw), [in_], [out], semkey=key)
        if key not in self.sem:
            self.sem[key] = self.free_sems.pop()
            self.count[key] = 0
        self.batch.setdefault(key, []).append(op)
        if prefetch:
            self.prefetch_keys.add(key)
        if last:
            self.count[key] += 16 * len(self.batch[key])
            for o in self.batch[key]:
                o.sigval = self.count[key]
            self.batch[key] = []
        return op

    def close(self, key):
        if self.batch.get(key):
            self.count[key] += 16 * len(self.batch[key])
            for o in self.batch[key]:
                o.sigval = self.count[key]
            self.batch[key] = []

    def flush(self):
        nc = self.nc
        assert all(len(v) == 0 for v in self.batch.values()), "open dma batch"
        ops = self.ops
        self.ops = []
        per = {e: [] for e in self.ENGS}
        for op in ops:
            per[op.eng].append(op)
        for e in self.ENGS:
            for op in reversed(per[e]):
                if op.semkey is None:
                    op.signal = True
                    break
        for e in self.ENGS:
            for op in per[e]:
                if op.semkey is None and op.signal:
                    self.count[e] += 1
                    op.sigval = self.count[e]
        final = {}
        for e in self.ENGS:
            final[e] = self.count[e]
        dma_final = {}
        for op in ops:
            if op.semkey is not None and op.semkey not in self.prefetch_keys:
                dma_final[op.semkey] = max(dma_final.get(op.semkey, 0), op.sigval)
        known = self.known
        sem = self.sem

        def emit(ename, eng):
            kn = known[ename]
            for op in per[ename]:
                need = {}
                for d in op.deps:
                    k = d.semkey if d.semkey is not None else d.eng
                    if d.sigval > need.get(k, 0):
                        need[k] = d.sigval
                for k, v in need.items():
                    if kn.get(k, 0) >= v:
                        continue
                    eng.wait_ge(sem[k], v)
                    kn[k] = v
                ins = op.fn(eng)
                if op.semkey is not None:
                    ins.then_inc(sem[op.semkey], 16)
                elif op.signal:
                    ins.then_inc(sem[ename], 1)
            for k, v in list(final.items()) + list(dma_final.items()):
                if k == ename or v == 0:
                    continue
                if kn.get(k, 0) >= v:
                    continue
                eng.wait_ge(sem[k], v)
                kn[k] = v

        with nc.Block() as block:
            @block.tensor
            def _(eng):
                emit('pe', eng)

            @block.vector
            def _(eng):
                emit('dve', eng)

            @block.scalar
            def _(eng):
                emit('act', eng)

            @block.gpsimd
            def _(eng):
                emit('pool', eng)

            @block.sync
            def _(eng):
                emit('sp', eng)
        for op in ops:
            if op.semkey is None or op.semkey not in self.prefetch_keys:
                op.fn = None
                op.deps = None
            else:
                op.deps = []
        self.prefetch_keys = set()
        self.nflush += 1


class Ctx:
    pass


NIT = 18
XD = 2944
XTOT = 3 * XD
SEG_A, SEG_D, SEG_C = 0, 1, 2


def _t5_bucket(dist):
    dist = np.maximum(dist, 0)
    d_f = np.maximum(dist, 1).astype(np.float32)
    large = 16 + (np.log(d_f / np.float32(16)) / np.float32(math.log(128.0)) * np.float32(16)).astype(np.int32)
    large = np.minimum(large, 31)
    return np.where(dist < 16, dist, large)


def _const_arrays():
    ident = np.eye(128, dtype=np.float32)
    d = np.arange(XD) - 511
    oh = np.zeros((32, XD), np.float32)
    oh[_t5_bucket(d), np.arange(XD)] = 1.0
    valid = np.zeros((16, XTOT), np.float32)
    ok = (d >= 0) & (d <= 2047)
    multA = ((d <= 128).astype(np.float32) + ((d % 4 == 0) & (d <= 512)).astype(np.float32)
             + ((d % 16 == 0) & (d <= 2048)).astype(np.float32)) * ok
    valid[:, SEG_A * XD:(SEG_A + 1) * XD] = multA[None]
    valid[:, SEG_D * XD:(SEG_D + 1) * XD] = ok.astype(np.float32)[None]
    valid[:, SEG_C * XD:(SEG_C + 1) * XD] = (ok & (d <= 127)).astype(np.float32)[None]
    t = np.arange(128)
    cneg = np.where(t[None, :] <= t[:, None], 0.0, -1e30).astype(np.float32)
    pow2 = np.tile((2.0 ** -(np.arange(NIT)))[None].astype(np.float32), (128, 1))
    masks = np.zeros((128, 8), np.float32)
    masks[0:64, 0] = 1.0
    masks[64:128, 1] = 1.0
    for i in range(4):
        masks[32 * i:32 * i + 32, 2 + i] = 1.0
    return {"c_ident": ident, "c_oh": oh, "c_valid": valid, "c_cneg": cneg, "c_pow2": pow2, "c_masks": masks}


def build(nseq, plan):
    nc = bass.Bass("TRN2", target_bir_lowering=False)
    ntok = nseq * SEQ
    dram = {}

    def din(name, shape, dt=F32):
        dram[name] = nc.dram_tensor(name, list(shape), dt, kind="ExternalInput").ap()
        return dram[name]

    x_d = din("x", (ntok, D))
    norm_g = din("norm_g", (2, 3, D))
    final_g = din("final_g", (1, D))
    table_d = din("rel_bias_table", (32, 16))
    w1_d = din("ffn_w1", (2, 2, D, DFF))
    w3_d = din("ffn_w3", (2, 2, D, DFF))
    w2_d = din("ffn_w2", (2, 2, DFF, D))
    hyb_w_in = din("hyb_w_in", (1, D, 2856))
    hyb_cq_g = din("hyb_cq_g", (1, 256))
    hyb_wq_b = din("hyb_wq_b", (1, 256, 512))
    hyb_wq_idx = din("hyb_wq_idx", (1, 256, 256))
    hyb_w_out = din("hyb_w_out", (1, D, D))
    swa_w_in = din("swa_w_in", (1, D, 1280))
    swa_sinks = din("swa_sinks", (1, 16))
    swa_w_out = din("swa_w_out", (1, D, D))
    c_ident = din("c_ident", (128, 128))
    c_oh = din("c_oh", (32, XD))
    c_valid = din("c_valid", (16, XTOT))
    c_cneg = din("c_cneg", (128, 128))
    c_pow2 = din("c_pow2", (128, NIT))
    c_masks = din("c_masks", (128, 8))
    Fscr = nc.dram_tensor("Fscr", [16, XTOT], BF16, kind="Internal").ap()
    Fscr2 = nc.dram_tensor("Fscr2", [128, 16 * XTOT], BF16, kind="Internal").ap()
    APc = type(x_d)
    y_d = nc.dram_tensor("y", [ntok, D], F32, kind="ExternalOutput").ap()

    from contextlib import ExitStack
    es = ExitStack()
    with es:
        def sb(name, shape, dt):
            return es.enter_context(nc.sbuf_tensor(name, list(shape), dt))

        def psum(name, shape, dt):
            return es.enter_context(nc.psum_tensor(name, list(shape), dt))

        sems = [es.enter_context(nc.semaphore("s%d" % i)) for i in range(40)]
        p = Prog(nc, sems)

        h = sb("h", (128, NCH, SEQ), F32)
        ident_f = sb("ident_f", (128, 128), F32)
        ident_b = sb("ident_b", (128, 128), BF16)
        ones_b = sb("ones_b", (128, 128), BF16)
        gains = sb("gains", (128, 7, NCH), F32)
        gfin_bc = sb("gfin_bc", (128, D), F32)
        wbuf = sb("wbuf", (128, 12288), BF16)
        oT = sb("oT", (128, NCH, SEQ), BF16)
        big = sb("big", (128, 30720), BF16)
        misc = sb("misc", (128, 3584), F32)
        ps = [psum("ps%d" % i, (128, 512), F32) for i in range(8)]

        p.dma('sp', ident_f[:], c_ident, 'c0', last=False)
        p.close('c0')
        p.copy('dve', ident_b[:], ident_f[:])
        p.memset('dve', ones_b[:], 1.0)
        for l in range(2):
            for n in range(3):
                p.dma('sp', gains[:, l * 3 + n, :],
                      norm_g[l, n].rearrange("(c q) -> q c", q=128), 'c1',
                      allow_slow_non_contiguous=True)
        p.dma('sp', gains[:, 6, :], final_g[0].rearrange("(c q) -> q c", q=128), 'c1', last=False,
              allow_slow_non_contiguous=True)
        p.dma('sp', gfin_bc[:], final_g.partition_broadcast(128), 'c1', last=False)
        small = sb("small", (128, 64), F32)
        esink = small[:, 0:8]
        table_sb = small[0:32, 16:32]
        cneg = sb("cneg", (128, 128), F32)
        pow2 = small[:, 32:32 + NIT]
        p.dma('sp', cneg[:], c_cneg, 'c1', last=False)
        p.dma('sp', pow2, c_pow2, 'c1', last=False)
        maskt = sb("maskt", (128, 8), F32)
        p.dma('sp', maskt[:], c_masks, 'c1', last=False)
        p.dma('sp', table_sb, table_d, 'c1', last=False)
        p.dma('sp', small[0:64, 0:8], APc(swa_sinks.tensor, 0, [[0, 64], [2, 8]]), 'c1', last=False,
              allow_slow_non_contiguous=True)
        p.dma('sp', small[64:128, 0:8], APc(swa_sinks.tensor, 1, [[0, 64], [2, 8]]), 'c1', last=False,
              allow_slow_non_contiguous=True)
        p.close('c1')
        p.act(esink, esink, AF.Exp)
        h2 = h[:].rearrange("q c t -> q (c t)")
        oh_sb = h2[0:32, 0:XD]
        va_sb = h2[0:16, XD:XD + XTOT]
        e0_sb = h2[0:16, XD + XTOT:2 * XD + XTOT]
        fb_sb = big[0:16, 0:XTOT]
        p.dma('sp', oh_sb, c_oh, 'c2', last=False)
        p.dma('sp', va_sb, c_valid, 'c2', last=False)
        p.close('c2')
        for i in range((XD + 511) // 512):
            w = min(512, XD - i * 512)
            bank = ps[i % 2]
            p.mm(bank[0:16, 0:w], table_sb, oh_sb[:, i * 512:i * 512 + w])
            p.act(e0_sb[:, i * 512:i * 512 + w], bank[0:16, 0:w], AF.Exp)
        for sg in range(3):
            p.tt('dve', fb_sb[:, sg * XD:(sg + 1) * XD], e0_sb, va_sb[:, sg * XD:(sg + 1) * XD], ALU.mult)
        p.dma('sp', Fscr, fb_sb, 'c3')
        p.dma('sp', Fscr2, APc(Fscr.tensor, 0, [[0, 128], [1, 16 * XTOT]]), 'c4')
        p.flush()

        def toeplitz(seg, thead, width):
            return APc(Fscr2.tensor, thead * XTOT + seg * XD + 127, [[16 * XTOT - 1, 128], [1, width]])

        def mview(off, n):
            return misc[:, off:off + n]

        rr = [0]

        def load_x(s):
            for blk in range(16):
                xin = mview((blk % 2) * 1024, 1024)
                p.dma('sp', xin, x_d[s * SEQ + blk * 128: s * SEQ + (blk + 1) * 128, :], 'xin%d' % (blk % 2))
                for half in range(2):
                    bank = ps[(blk * 2 + half) % 4]
                    for i in range(4):
                        c = half * 4 + i
                        p.tr(bank[:, i * 128:(i + 1) * 128], xin[:, c * 128:(c + 1) * 128], ident_f[:])
                    dst = h[:, half * 4:half * 4 + 4, blk * 128:(blk + 1) * 128]
                    src = bank[:].rearrange("q (a t) -> q a t", a=4)
                    p.copy('act' if (blk + half) % 2 else 'dve', dst, src)
            p.flush()

        def rms_rstd(tok0, n, rstd_out, work_bf, work_f):
            bank = ps[7]
            for c in range(NCH):
                sq = work_bf[c % 2]
                p.act(sq, h[:, c, tok0:tok0 + 512], AF.Square)
                p.mm(bank[:], ones_b[:], sq, start=(c == 0), stop=(c == NCH - 1))
            p.act(work_f, bank[:], AF.Ln, scale=1.0 / D, bias=eps_t[:, 0:1])
            p.act(rstd_out, work_f, AF.Exp, scale=-0.5)

        eps_t = sb("eps_t", (128, 1), F32)
        p.memset('dve', eps_t[:], EPS)

        def ffn(l, j):
            n = l * 3 + (0 if j == 0 else 2)
            w1v = w1_d[l, j].rearrange("(c q) f -> q c f", q=128)
            w3v = w3_d[l, j].rearrange("(c q) f -> q c f", q=128)
            w2v = w2_d[l, j].rearrange("(f q) o -> q f o", q=128)
            xn = big[:, 0:8192].rearrange("q (c t) -> q c t", c=NCH)
            hT = big[:, 8192:8192 + 22528].rearrange("q (f t) -> q f t", f=NFC)
            sqw = [misc[:, 2048:2304].bitcast(BF16), misc[:, 2304:2560].bitcast(BF16)]
            lnw = mview(2560, 512)
            rstd = mview(3072, 512)
            sA = [mview(1024, 512), mview(1536, 512)]
            groups = [(g * 384, min(384, DFF - g * 384)) for g in range(8)]

            def upslot(g, which):
                s = (g % 2) * 2 + which
                return wbuf[:, s * 3072:(s + 1) * 3072].rearrange("q (c f) -> q c f", c=NCH)

            def w2slot(g):
                s = g % 2
                return wbuf[:, s * 5632:(s + 1) * 5632].rearrange("q (f o) -> q f o", f=NFC)

            for half in range(2):
                T0 = half * 1024

                def load_up(g):
                    f0, fw = groups[g]
                    p.dma('pool', upslot(g, 0)[:, :, 0:fw], w1v[:, :, f0:f0 + fw], 'wu%d' % (g % 2), last=False)
                    p.dma('pool', upslot(g, 1)[:, :, 0:fw], w3v[:, :, f0:f0 + fw], 'wu%d' % (g % 2), last=True)

                def load_dn(g):
                    p.dma('pool', w2slot(g), w2v[:, :, g * 256:(g + 1) * 256], 'wd%d' % (g % 2))

                load_up(0)
                load_up(1)
                for ts in range(2):
                    t0 = T0 + ts * 512
                    rms_rstd(t0, n, rstd, sqw, lnw)
                    for c in range(NCH):
                        p.stt(xn[:, c, ts * 512:(ts + 1) * 512], h[:, c, t0:t0 + 512],
                              gains[:, n, c:c + 1], rstd, ALU.mult, ALU.mult)
                k = 0
                for g in range(8):
                    f0, fw = groups[g]
                    for fi in range(fw // 128):
                        f = (f0 // 128) + fi
                        for ts in range(2):
                            bA = ps[(k % 2) * 2]
                            bB = ps[(k % 2) * 2 + 1]
                            for c in range(NCH):
                                p.mm(bA[:], upslot(g, 0)[:, c, fi * 128:(fi + 1) * 128],
                                     xn[:, c, ts * 512:(ts + 1) * 512], start=(c == 0), stop=(c == NCH - 1))
                            for c in range(NCH):
                                p.mm(bB[:], upslot(g, 1)[:, c, fi * 128:(fi + 1) * 128],
                                     xn[:, c, ts * 512:(ts + 1) * 512], start=(c == 0), stop=(c == NCH - 1))
                            p.act(sA[k % 2], bA[:], AF.Silu)
                            p.tt('dve', hT[:, f, ts * 512:(ts + 1) * 512], bB[:], sA[k % 2], ALU.mult)
                            k += 1
                    if g + 2 < 8:
                        load_up(g + 2)
                    elif g == 6:
                        load_dn(0)
                    elif g == 7:
                        load_dn(1)
                k = 0
                for g in range(4):
                    for oc in range(2):
                        o = g * 2 + oc
                        for ts in range(2):
                            bank = ps[4 + (k % 2)]
                            for f in range(NFC):
                                p.mm(bank[:], w2slot(g)[:, f, oc * 128:(oc + 1) * 128],
                                     hT[:, f, ts * 512:(ts + 1) * 512], start=(f == 0), stop=(f == NFC - 1))
                            hs = h[:, o, T0 + ts * 512:T0 + (ts + 1) * 512]
                            p.stt(hs, bank[:], 0.5, hs, ALU.mult, ALU.add)
                            k += 1
                    if g + 2 < 4:
                        load_dn(g + 2)
                p.flush()


        def load_w_cast(dst, src_ap, key, last=True):
            p.dma('pool', dst, src_ap, key, last=last, max_dma_last_dim=4096)

        def evac(i, out, in_):
            full = (in_.shape[0] == 128 and in_.shape[-1] == 512)
            p.copy('act' if (i % 2 and full) else 'dve', out, in_)

        xtra = sb("xtra", (128, 3072), BF16)
        Et = [big[:, 28672:29184], big[:, 29184:29696]]
        stripb = [wbuf[:, 0:2816], wbuf[:, 2816:5632]]
        qmt = [xtra[:, 0:512], xtra[:, 512:1024]]
        qim = [xtra[:, 2048:2176], xtra[:, 2176:2304]]

        def norm_tile(t0, n, u_t):
            sqw = [misc[:, 2048:2304].bitcast(BF16), misc[:, 2304:2560].bitcast(BF16)]
            lnw = mview(2560, 512)
            rstd = mview(3072, 512)
            rms_rstd(t0, n, rstd, sqw, lnw)
            for c in range(NCH):
                p.stt(u_t[:, c, :], h[:, c, t0:t0 + 512], gains[:, n, c:c + 1], rstd, ALU.mult, ALU.mult)

        kctr = [0]

        def proj_fm(dst, wsl, u_t, nk=NCH):
            k = kctr[0]
            kctr[0] += 1
            bank = ps[k % 4]
            M = dst.shape[0]
            N = dst.shape[-1]
            for c in range(nk):
                p.mm(bank[0:M, 0:N], wsl(c), u_t[:, c, :], start=(c == 0), stop=(c == nk - 1))
            evac(k, dst, bank[0:M, 0:N])

        def toep_attn(nheads, QW, spec, jrange, seg, selT=None, pre_q=None):
            nQ = SEQ // QW
            k = 0
            import os as _os
            lvl = int(_os.environ.get("K_LVL", "9"))
            for Q in range(nQ):
                if pre_q is not None:
                    pre_q(Q)
                js = jrange(Q)
                for hd in range(nheads):
                    import os as _os
                    if _os.environ.get("K_EVEN") == "1" and hd % 2 == 1:
                        continue
                    if _os.environ.get("K_EVEN") == "2" and hd % 2 == 0:
                        continue
                    sp_ = spec(hd)
                    r0 = sp_['r0']
                    strip = stripb[k % 2]
                    ymin = QW * Q - 128 * js[-1] + 384
                    ymax = QW * Q - 128 * js[0] + 384 + QW
                    if _os.environ.get("K_NOSTRIP") == "1":
                        p.memset('dve', strip[:, ymin:ymax], 1.0)
                    else:
                        p.dma('sp', strip[:, ymin:ymax],
                              APc(Fscr2.tensor, sp_['thead'] * XTOT + seg * XD + 127 + ymin,
                                  [[16 * XTOT - 1, 128], [1, ymax - ymin]]), 'st%d' % (k % 2))
                    psn = ps[4 + 2 * (k % 2)]
                    psd = ps[5 + 2 * (k % 2)]
                    q_ap = qmt[k % 2][:, 0:QW]
                    p.ts('dve', q_ap, sp_['q'](Q), maskt[:, r0 // 64:r0 // 64 + 1], ALU.mult)
                    for idx, j in enumerate(js):
                        sbank = ps[idx % 4]
                        E = Et[idx % 2][:, 0:QW]
                        y0 = QW * Q - 128 * j + 384
                        sub = _os.environ.get("K_SUB", "z")
                        if sub >= "b":
                            p.mm(sbank[:, 0:QW], sp_['k'](j), q_ap)
                        if sub >= "c":
                            p.act(E, sbank[:, 0:QW], AF.Exp, scale=0.125)
                        if sub >= "d":
                            p.tt('dve', E, E, strip[:, y0:y0 + QW], ALU.mult)
                        if selT is not None:
                            p.tt('dve', E, E, selT[:, j, :], ALU.mult)
                        if lvl >= 2:
                            p.mm(psn[:, 0:QW], sp_['v'](j), E, start=(idx == 0), stop=(idx == len(js) - 1))
                            p.mm(psd[:, 0:QW], ones_b[:, :], E, start=(idx == 0), stop=(idx == len(js) - 1))
                    if lvl < 3:
                        return
                    lnd = mview(3072 + 0, 512)[r0:r0 + 64, 0:QW]
                    if sp_.get('sink') is not None:
                        p.act(lnd, psd[r0:r0 + 64, 0:QW], AF.Ln, bias=sp_['sink'])
                    else:
                        p.act(lnd, psd[r0:r0 + 64, 0:QW], AF.Ln)
                    p.act(lnd, lnd, AF.Exp, scale=-1.0)
                    p.tt('dve', oT[r0:r0 + 64, sp_['oc'], Q * QW:(Q + 1) * QW], psn[r0:r0 + 64, 0:QW], lnd, ALU.mult)
                    k += 1

        wi_sb = sb("wi_sb", (128, 128), F32)
        bis = sb("bis", (128, 32), F32)

        def dsa(l, win):
            n = l * 3 + 1
            li = l // 2
            kbT = big[:, 0:8192].rearrange("q (c t) -> q c t", c=4)
            vb = big[:, 8192:16384].rearrange("q (b f) -> q b f", b=16)
            cqT = big[:, 16384:20480].rearrange("q (c t) -> q c t", c=2)
            kidx3 = big[:, 20480:22528]
            sel = big[:, 22528:24576]
            u_t = big[:, 24576:28672].rearrange("q (c t) -> q c t", c=8)
            qbt = big[:, 24576:25600].rearrange("q (c t) -> q c t", c=4)
            qit = big[:, 25600:26112].rearrange("q (c t) -> q c t", c=2)
            wcq = wbuf[:, 0:2048].rearrange("q (c f) -> q c f", c=8)
            wkb = wbuf[:, 2048:6144].rearrange("q (c f) -> q c f", c=8)
            wvb = wbuf[:, 6144:10240].rearrange("q (c f) -> q c f", c=8)
            wki = wbuf[:, 10240:11264].rearrange("q (c f) -> q c f", c=8)
            wwi = wbuf[:, 11264:11328].rearrange("q (c f) -> q c f", c=8)
            selT = wbuf[:, 5632:9728].rearrange("q (j t) -> q j t", j=16)
            wqb = wbuf[:, 9728:10752].rearrange("q (c f) -> q c f", c=2)
            wqi = wbuf[:, 10752:11264].rearrange("q (c f) -> q c f", c=2)
            gcq = small[:, 56:58]
            sc = mview(0, 2048)
            rt = [mview(2048, 512), mview(2560, 512)]
            sqw = [misc[:, 2048:2304].bitcast(BF16), misc[:, 2304:2560].bitcast(BF16)]
            lnw = mview(2560, 512)
            rstd = mview(3072, 512)
            load_w_cast(wcq, win[:, :, 1536:1792], 'wm0', last=False)
            load_w_cast(wkb, win[:, :, 1792:2304], 'wm0', last=False)
            load_w_cast(wvb, win[:, :, 2304:2816], 'wm0', last=False)
            for r in range(4):
                load_w_cast(wki[:, :, r * 32:(r + 1) * 32], win[:, :, 2816:2848], 'wm0', last=False)
            load_w_cast(wwi, win[:, :, 2848:2856], 'wm0', last=True)
            p.dma('sp', gcq, hyb_cq_g[li].rearrange("(c q) -> q c", q=128), 'cg0', allow_slow_non_contiguous=True)
            for tt in range(4):
                t0 = tt * 512
                norm_tile(t0, n, u_t)
                for oc in range(2):
                    proj_fm(cqT[:, oc, t0:t0 + 512], lambda c, oc=oc: wcq[:, c, oc * 128:(oc + 1) * 128], u_t)
                for oc in range(4):
                    proj_fm(kbT[:, oc, t0:t0 + 512], lambda c, oc=oc: wkb[:, c, oc * 128:(oc + 1) * 128], u_t)
                proj_fm(kidx3[:, t0:t0 + 512], lambda c: wki[:, c, :], u_t)
                for bi in range(4):
                    kk = kctr[0]
                    kctr[0] += 1
                    bank = ps[kk % 4]
                    for c in range(NCH):
                        p.mm(bank[:], u_t[:, c, bi * 128:(bi + 1) * 128], wvb[:, c, :], start=(c == 0), stop=(c == 7))
                    evac(kk, vb[:, tt * 4 + bi, :], bank[:])
                kk = kctr[0]
                kctr[0] += 1
                bank = ps[kk % 4]
                for bi in range(4):
                    for c in range(NCH):
                        p.mm(bank[:, bi * 8:(bi + 1) * 8], u_t[:, c, bi * 128:(bi + 1) * 128], wwi[:, c, :],
                             start=(c == 0), stop=(c == 7))
                p.copy('dve', wi_sb[:, tt * 32:(tt + 1) * 32], bank[:, 0:32])
                b7 = ps[7]
                for c in range(2):
                    p.act(sqw[c], cqT[:, c, t0:t0 + 512], AF.Square)
                    p.mm(b7[:], ones_b[:], sqw[c], start=(c == 0), stop=(c == 1))
                p.act(lnw, b7[:], AF.Ln, scale=1.0 / 256, bias=eps_t[:, 0:1])
                p.act(rstd, lnw, AF.Exp, scale=-0.5)
                for c in range(2):
                    p.stt(cqT[:, c, t0:t0 + 512], cqT[:, c, t0:t0 + 512], gcq[:, c:c + 1], rstd, ALU.mult, ALU.mult)
            p.flush()
            wqbv = hyb_wq_b[li].rearrange("(c q) f -> q c f", q=128)
            wqiv = hyb_wq_idx[li].rearrange("(c q) f -> q c f", q=128)
            load_w_cast(wqb, wqbv, 'wm0', last=False)
            load_w_cast(wqi, wqiv, 'wm0', last=True)
            QW = 256
            psb = ps[3][:].bitcast(BF16)
            qimh = [xtra[:, 1024 + i * 128:1024 + (i + 1) * 128] for i in range(8)]

            def select_block(Q, bl):
                b = 2 * Q + bl
                nk = (b + 1) * 128
                qcols = slice(bl * 128, (bl + 1) * 128)
                for kc in range((nk + 511) // 512):
                    w = min(512, nk - kc * 512)
                    scs = sc[:, kc * 512:kc * 512 + w]
                    for hi in range(8):
                        g = hi // 4
                        kk = kctr[0]
                        kctr[0] += 1
                        bank = ps[kk % 3]
                        if kc == 0:
                            p.ts('dve', qimh[hi], qit[:, g, qcols], maskt[:, 2 + hi % 4:3 + hi % 4], ALU.mult)
                        p.mm(bank[:, 0:w], qimh[hi], kidx3[:, kc * 512:kc * 512 + w])
                        rtk = rt[kk % 2][:, 0:w]
                        p.act(rtk, bank[:, 0:w], AF.Relu)
                        wcol = wi_sb[:, b * 8 + hi:b * 8 + hi + 1]
                        if hi == 0:
                            p.ts('dve', scs, rtk, wcol, ALU.mult)
                        else:
                            p.stt(scs, rtk, wcol, scs, ALU.mult, ALU.add)
                A = bis[:, 0:1]
                lo = bis[:, 1:2]
                mid = bis[:, 2:3]
                cnt = bis[:, 3:4]
                tmp = bis[:, 4:5]
                steps = bis[:, 8:8 + NIT]
                if b >= 2:
                    p.reduce(A, sc[:, 0:nk], ALU.max, absval=True)
                p.tt('dve', sc[:, b * 128:nk], sc[:, b * 128:nk], cneg[:], ALU.add)
                if b < 2:
                    p.ts('dve', sel[:, 0:nk], sc[:, 0:nk], -1e29, ALU.is_gt)
                else:
                    p.ts('dve', steps, pow2, A, ALU.mult)
                    p.ts('dve', lo, A, -1.0, ALU.mult)
                    for it in range(NIT):
                        p.tt('dve', mid, lo, steps[:, it:it + 1], ALU.add)
                        p.ts('dve', sel[:, 0:nk], sc[:, 0:nk], mid, ALU.is_ge, 0.0, ALU.add, accum_out=cnt)
                        p.ts('dve', tmp, cnt, 255.5, ALU.is_gt, steps[:, it:it + 1], ALU.mult)
                        p.tt('dve', lo, lo, tmp, ALU.add)
                    p.ts('dve', sel[:, 0:nk], sc[:, 0:nk], lo, ALU.is_ge)
                for jb0 in range(0, b + 1, 8):
                    nb = min(8, b + 1 - jb0)
                    for i in range(nb):
                        p.tr(psb[:, i * 128:(i + 1) * 128], sel[:, (jb0 + i) * 128:(jb0 + i + 1) * 128], ident_b[:])
                    for i in range(nb):
                        p.copy('dve', selT[:, jb0 + i, qcols], psb[:, i * 128:(i + 1) * 128])

            def pre_q(Q):
                cq_t = cqT[:, :, Q * QW:(Q + 1) * QW]
                for oc in range(4):
                    proj_fm(qbt[:, oc, :], lambda c, oc=oc: wqb[:, c, oc * 128:(oc + 1) * 128], cq_t, nk=2)
                for g in range(2):
                    proj_fm(qit[:, g, :], lambda c, g=g: wqi[:, c, g * 128:(g + 1) * 128], cq_t, nk=2)
                p.memset('dve', selT[:, 2 * Q + 1, 0:128], 0.0)
                for bl in range(2):
                    select_block(Q, bl)

            def specB(hd):
                c, r0 = hd // 2, 64 * (hd % 2)
                return dict(r0=r0, thead=8 + hd, oc=4 + c, sink=None,
                            q=lambda Q: qbt[:, c, :],
                            k=lambda j: kbT[:, c, j * 128:(j + 1) * 128],
                            v=lambda j: vb[:, j, c * 128:(c + 1) * 128])

            toep_attn(8, QW, specB, lambda Q: list(range(0, 2 * Q + 2)), SEG_D, selT=selT, pre_q=pre_q)
            p.flush()

        def swa_mixer(l):
            n = l * 3 + 1
            li = l // 2
            win = swa_w_in[li].rearrange("(c q) f -> q c f", q=128)
            wout = swa_w_out[li].rearrange("(c q) f -> q c f", q=128)
            qT = big[:, 0:16384].rearrange("q (c t) -> q c t", c=8)
            kT = [big[:, 16384:18432], big[:, 18432:20480]]
            V = [big[:, 20480:22528].rearrange("q (b f) -> q b f", b=16),
                 big[:, 22528:24576].rearrange("q (b f) -> q b f", b=16)]
            u_t = big[:, 24576:28672].rearrange("q (c t) -> q c t", c=8)
            wq = wbuf[:, 0:8192].rearrange("q (c f) -> q c f", c=8)
            wk = wbuf[:, 8192:10240].rearrange("q (c f) -> q c f", c=8)
            wv = wbuf[:, 10240:12288].rearrange("q (c f) -> q c f", c=8)
            load_w_cast(wq, win[:, :, 0:1024], 'wm0', last=False)
            load_w_cast(wk[:, :, 0:128], win[:, :, 1024:1152], 'wm0', last=False)
            load_w_cast(wk[:, :, 128:192], win[:, :, 1088:1152], 'wm0', last=False)
            load_w_cast(wk[:, :, 192:256], win[:, :, 1024:1088], 'wm0', last=False)
            load_w_cast(wv[:, :, 0:128], win[:, :, 1152:1280], 'wm0', last=False)
            load_w_cast(wv[:, :, 128:192], win[:, :, 1216:1280], 'wm0', last=False)
            load_w_cast(wv[:, :, 192:256], win[:, :, 1152:1216], 'wm0', last=True)
            import os as _os
            dbg = _os.environ.get("K_DBG", "")
            for tt in range(4):
                t0 = tt * 512
                if dbg == "w":
                    break
                norm_tile(t0, n, u_t)
                if dbg == "n":
                    continue
                for oc in range(8):
                    proj_fm(qT[:, oc, t0:t0 + 512], lambda c, oc=oc: wq[:, c, oc * 128:(oc + 1) * 128], u_t)
                if dbg == "q":
                    continue
                for v_ in range(2):
                    proj_fm(kT[v_][:, t0:t0 + 512], lambda c, v_=v_: wk[:, c, v_ * 128:(v_ + 1) * 128], u_t)
                if dbg == "k":
                    continue
                for v_ in range(2):
                    kk = kctr[0]
                    kctr[0] += 1
                    bank = ps[kk % 4]
                    for bi in range(4):
                        for c in range(NCH):
                            p.mm(bank[:, bi * 128:(bi + 1) * 128], u_t[:, c, bi * 128:(bi + 1) * 128],
                                 wv[:, c, v_ * 128:(v_ + 1) * 128], start=(c == 0), stop=(c == 7))
                    p.copy('dve', big[:, 20480 + v_ * 2048 + tt * 512:20480 + v_ * 2048 + (tt + 1) * 512], bank[:])
            if dbg in ("w", "n", "q", "k", "v"):
                p.flush()
                return

            def spec(hd):
                c, r0, g = hd // 2, 64 * (hd % 2), hd // 8
                v_ = 0 if 64 * g == r0 else 1
                return dict(r0=r0, thead=hd, oc=c, sink=esink[r0:r0 + 64, c:c + 1],
                            q=lambda Q: qT[:, c, Q * 512:(Q + 1) * 512],
                            k=lambda j: kT[v_][:, j * 128:(j + 1) * 128],
                            v=lambda j: V[v_][:, j, :])

            import os as _os
            dbg = _os.environ.get("K_DBG", "")
            if dbg == "proj":
                p.flush()
                return
            toep_attn(16, 512, spec, lambda Q: list(range(max(0, 4 * Q - 1), 4 * Q + 4)), SEG_C)
            p.flush()
            if dbg == "attn":
                return
            out_proj(wout)

        def hyb_mixer(l, do_b=True):
            n = l * 3 + 1
            li = l // 2
            win = hyb_w_in[li].rearrange("(c q) f -> q c f", q=128)
            wout = hyb_w_out[li].rearrange("(c q) f -> q c f", q=128)
            u_t = big[:, 24576:28672].rearrange("q (c t) -> q c t", c=8)
            qT = big[:, 0:8192].rearrange("q (c t) -> q c t", c=4)
            kT = big[:, 8192:16384].rearrange("q (c t) -> q c t", c=4)
            V = big[:, 16384:24576].rearrange("q (b f) -> q b f", b=16)
            wq = wbuf[:, 0:4096].rearrange("q (c f) -> q c f", c=8)
            wk = wbuf[:, 4096:8192].rearrange("q (c f) -> q c f", c=8)
            wv = wbuf[:, 8192:12288].rearrange("q (c f) -> q c f", c=8)
            load_w_cast(wq, win[:, :, 0:512], 'wm0', last=False)
            load_w_cast(wk, win[:, :, 512:1024], 'wm0', last=False)
            load_w_cast(wv, win[:, :, 1024:1536], 'wm0', last=True)
            for tt in range(4):
                t0 = tt * 512
                norm_tile(t0, n, u_t)
                for oc in range(4):
                    proj_fm(qT[:, oc, t0:t0 + 512], lambda c, oc=oc: wq[:, c, oc * 128:(oc + 1) * 128], u_t)
                    proj_fm(kT[:, oc, t0:t0 + 512], lambda c, oc=oc: wk[:, c, oc * 128:(oc + 1) * 128], u_t)
                for bi in range(4):
                    kk = kctr[0]
                    kctr[0] += 1
                    bank = ps[kk % 4]
                    for c in range(NCH):
                        p.mm(bank[:], u_t[:, c, bi * 128:(bi + 1) * 128], wv[:, c, :], start=(c == 0), stop=(c == 7))
                    evac(kk, V[:, tt * 4 + bi, :], bank[:])

            def specA(hd):
                c, r0 = hd // 2, 64 * (hd % 2)
                return dict(r0=r0, thead=hd, oc=c, sink=None,
                            q=lambda Q: qT[:, c, Q * 512:(Q + 1) * 512],
                            k=lambda j: kT[:, c, j * 128:(j + 1) * 128],
                            v=lambda j: V[:, j, c * 128:(c + 1) * 128])

            toep_attn(8, 512, specA, lambda Q: list(range(0, 4 * Q + 4)), SEG_A)
            p.flush()
            if not do_b:
                for c in range(4, 8):
                    p.memset('dve', oT[:, c, :], 0.0)
                p.flush()
                out_proj(wout)
                return
            dsa(l, win)
            out_proj(wout)

        def out_proj(wout):
            wo = wbuf[:, 0:8192].rearrange("q (c f) -> q c f", c=8)
            load_w_cast(wo, wout, 'wm0')
            k = 0
            for tt in range(4):
                for o in range(8):
                    bank = ps[k % 4]
                    for c in range(NCH):
                        p.mm(bank[:], wo[:, c, o * 128:(o + 1) * 128], oT[:, c, tt * 512:(tt + 1) * 512],
                             start=(c == 0), stop=(c == 7))
                    hs = h[:, o, tt * 512:(tt + 1) * 512]
                    p.tt('dve', hs, bank[:], hs, ALU.add)
                    k += 1
            p.flush()

        def final_store(s, do_norm=True):
            for blk in range(16):
                ot = mview((blk % 2) * 1024, 1024)
                ssq = mview(2048 + (blk % 2) * 8, 1)
                rs = mview(2064 + (blk % 2) * 8, 1)
                junk = mview(2560, 1024)
                for half in range(2):
                    bank = ps[(blk * 2 + half) % 4]
                    for i in range(4):
                        c = half * 4 + i
                        p.tr(bank[:, i * 128:(i + 1) * 128], h[:, c, blk * 128:(blk + 1) * 128], ident_f[:])
                    p.copy('act' if half else 'dve', ot[:, half * 512:(half + 1) * 512], bank[:])
                if do_norm:
                    p.act(junk, ot, AF.Square, accum_out=ssq)
                    p.act(rs, ssq, AF.Ln, scale=1.0 / D, bias=eps_t[:, 0:1])
                    p.act(rs, rs, AF.Exp, scale=-0.5)
                    p.stt(ot, ot, rs, gfin_bc[:], ALU.mult, ALU.mult)
                p.dma('sp', y_d[s * SEQ + blk * 128: s * SEQ + (blk + 1) * 128, :], ot, 'yo%d' % (blk % 2))
            p.flush()

        for s in range(nseq):
            load_x(s)
            for step in plan:
                if step[0] == 'ffn':
                    ffn(step[1], step[2])
                elif step[0] == 'swa':
                    swa_mixer(step[1])
                elif step[0] == 'hyb':
                    hyb_mixer(step[1])
                elif step[0] == 'hyba':
                    hyb_mixer(step[1], do_b=False)
            final_store(s, do_norm=('nofinal' not in [st[0] for st in plan]))
    return nc


FULL_PLAN = [('ffn', 0, 0), ('hyb', 0), ('ffn', 0, 1), ('ffn', 1, 0), ('swa', 1), ('ffn', 1, 1)]
PLAN = FULL_PLAN

_NC_CACHE = {}


def kernel(x, norm_g, final_g, rel_bias_table, ffn_w1, ffn_w3, ffn_w2, hyb_w_in, hyb_cq_g,
           hyb_wq_b, hyb_wq_idx, hyb_w_out, swa_w_in, swa_sinks, swa_w_out):
    f = lambda a: np.ascontiguousarray(np.asarray(a, dtype=np.float32))
    x = f(x)
    B = x.shape[0]
    per = B // NCORES
    consts = _const_arrays()
    shared = {
        "norm_g": f(norm_g), "final_g": f(final_g).reshape(1, D), "rel_bias_table": f(rel_bias_table),
        "ffn_w1": f(ffn_w1), "ffn_w3": f(ffn_w3), "ffn_w2": f(ffn_w2),
        "hyb_w_in": f(hyb_w_in), "hyb_cq_g": f(hyb_cq_g), "hyb_wq_b": f(hyb_wq_b),
        "hyb_wq_idx": f(hyb_wq_idx), "hyb_w_out": f(hyb_w_out),
        "swa_w_in": f(swa_w_in), "swa_sinks": f(swa_sinks), "swa_w_out": f(swa_w_out),
    }
    shared.update(consts)
    nc = build(per, PLAN)
    in_maps = []
    for i in range(NCORES):
        m = dict(shared)
        m["x"] = x[i * per:(i + 1) * per].reshape(per * SEQ, D)
        in_maps.append(m)
    res = run_bass_kernel_spmd(nc, in_maps, core_ids=list(range(NCORES)))
    outs = [np.asarray(r["y"]).reshape(per, SEQ, D) for r in res.results]
    return np.concatenate(outs, axis=0).astype(np.float32)
```
